# Optimizing a Trainium2 kernel written in Bass

```python
import jax, jax.numpy as jnp
from jax import lax
import numpy as np

D_MODEL = 1024
BATCH = 2
SEQ = 8192
DEPTH = 4

N_MEM = 256
D_FF = 2816
N_BRANCH = 3
NORM_EPS = 1e-6
MLA_HEADS = 8
MLA_NOPE = 64
MLA_ROPE = 32
MLA_V = 64
MLA_Q_RANK = 256
MLA_KV_RANK = 128
ROPE_THETA = 10000.0
Q_BLOCK = 128
MLA_W = MLA_HEADS * MLA_V
GLA_HEADS = 4
GLA_DK = 64
GLA_DV = 128
GLA_GATE_RANK = 16
GLA_GATE_NORM = 16.0
GLA_CHUNK = 64
GLA_W = GLA_HEADS * GLA_DV
RWKV_HEADS = 8
RWKV_N = 64
RWKV_DECAY_RANK = 64
RWKV_ICLR_RANK = 64
RWKV_GATE_RANK = 160
RWKV_GN_EPS = 64e-5
RWKV_W = RWKV_HEADS * RWKV_N
RWKV_COLS = 3 * RWKV_W + RWKV_DECAY_RANK + RWKV_ICLR_RANK + RWKV_GATE_RANK
MEM_HEADS = 4
MEM_HD = D_MODEL // MEM_HEADS
IN_SPLITS = (MLA_Q_RANK, MLA_KV_RANK, MLA_ROPE,
             GLA_HEADS * GLA_DK, GLA_HEADS * GLA_DK, GLA_W, GLA_W, GLA_GATE_RANK,
             RWKV_COLS, N_BRANCH * D_MODEL)
D_IN = sum(IN_SPLITS)
BRANCH_W = MLA_W + GLA_W + RWKV_W

kernel_name = "hybrid_mla_gla_rwkv7_gated_trunk"


def _offsets(sizes):
    out, acc = [], 0
    for s in sizes[:-1]:
        acc += s
        out.append(acc)
    return out


def _rmsnorm(x, g, eps=NORM_EPS):
    xf = x.astype(jnp.float32)
    y = xf * lax.rsqrt(jnp.mean(xf * xf, axis=-1, keepdims=True) + eps)
    return y.astype(x.dtype) * g


def _rope(t, cos, sin):
    t1, t2 = jnp.split(t, 2, axis=-1)
    return jnp.concatenate([t1 * cos - t2 * sin, t1 * sin + t2 * cos], axis=-1).astype(t.dtype)


def _swiglu(h, w_in, w_out):
    g, u = jnp.split(h @ w_in, 2, axis=-1)
    return (jax.nn.silu(g) * u) @ w_out


def _mla_branch(c_q, c_kv, k_rope_raw, q_norm, w_uq, kv_norm, w_ukv, cos, sin):
    B, S, _ = c_q.shape
    H = MLA_HEADS
    q = (_rmsnorm(c_q, q_norm) @ w_uq).reshape(B, S, H, MLA_NOPE + MLA_ROPE)
    q_nope = q[..., :MLA_NOPE]
    q_rot = _rope(q[..., MLA_NOPE:], cos[:, :, None, :], sin[:, :, None, :])
    kv = (_rmsnorm(c_kv, kv_norm) @ w_ukv).reshape(B, S, H, MLA_NOPE + MLA_V)
    k_nope, v = kv[..., :MLA_NOPE], kv[..., MLA_NOPE:]
    k_rot = _rope(k_rope_raw, cos, sin)
    scale = (MLA_NOPE + MLA_ROPE) ** -0.5
    nb = S // Q_BLOCK
    qn_b = q_nope.reshape(B, nb, Q_BLOCK, H, MLA_NOPE).swapaxes(0, 1)
    qr_b = q_rot.reshape(B, nb, Q_BLOCK, H, MLA_ROPE).swapaxes(0, 1)
    k_pos = jnp.arange(S)

    def block(args):
        qn, qr, blk = args
        s = (jnp.einsum('bqhd,bkhd->bhqk', qn, k_nope)
             + jnp.einsum('bqhr,bkr->bhqk', qr, k_rot)).astype(jnp.float32) * scale
        q_pos = blk * Q_BLOCK + jnp.arange(Q_BLOCK)
        s = jnp.where(k_pos[None, :] <= q_pos[:, None], s, -1e30)
        p = jax.nn.softmax(s, axis=-1).astype(v.dtype)
        return jnp.einsum('bhqk,bkhd->bqhd', p, v)

    o = lax.map(block, (qn_b, qr_b, jnp.arange(nb)))
    return o.swapaxes(0, 1).reshape(B, S, MLA_W)


def _gla_branch(q, k, v, g, a_lr, w_alpha, b_alpha, norm_g):
    B, S, _ = q.shape
    H, C = GLA_HEADS, GLA_CHUNK
    nc = S // C
    q = q.reshape(B, S, H, GLA_DK) * GLA_DK ** -0.5
    k = k.reshape(B, S, H, GLA_DK)
    v = v.reshape(B, S, H, GLA_DV)
    log_a = (jax.nn.log_sigmoid((a_lr @ w_alpha + b_alpha).astype(jnp.float32))
             / GLA_GATE_NORM).reshape(B, S, H, GLA_DK)

    def to_chunks(t):
        return t.reshape(B, nc, C, H, t.shape[-1]).transpose(1, 0, 3, 2, 4)

    causal = jnp.tril(jnp.ones((C, C), dtype=bool))[:, :, None]

    def step(state, inp):
        qc, kc, vc, lac = inp
        b = jnp.cumsum(lac, axis=2)
        b_last = b[:, :, -1:, :]
        diff = b[:, :, :, None, :] - b[:, :, None, :, :]
        decay = jnp.exp(jnp.where(causal, diff, -jnp.inf))
        attn = jnp.einsum('bhid,bhjd,bhijd->bhij', qc, kc, decay)
        o = (jnp.einsum('bhij,bhjv->bhiv', attn, vc)
             + jnp.einsum('bhid,bhdv->bhiv', qc * jnp.exp(b), state))
        state = (state * jnp.exp(b_last)[:, :, 0, :, None]
                 + jnp.einsum('bhjd,bhjv->bhdv', kc * jnp.exp(b_last - b), vc))
        return state, o

    s0 = jnp.zeros((B, H, GLA_DK, GLA_DV), jnp.float32)
    _, o = lax.scan(step, s0, (to_chunks(q), to_chunks(k), to_chunks(v), to_chunks(log_a)))
    o = o.transpose(1, 0, 3, 2, 4).reshape(B, S, H, GLA_DV).astype(v.dtype)
    o = _rmsnorm(o, norm_g).reshape(B, S, GLA_W)
    return o * jax.nn.silu(g)


def _rwkv_branch(z, mu, w0, w_decay, a0, w_iclr, w_gate, k_k, k_a, r_k, ln_w, ln_b):
    B, S, _ = z.shape
    H, N = RWKV_HEADS, RWKV_N
    z_prev = jnp.pad(z, ((0, 0), (1, 0), (0, 0)))[:, :-1]
    z = z + mu * (z_prev - z)
    r, k, v, xw, xa, xg = jnp.split(
        z, _offsets((RWKV_W, RWKV_W, RWKV_W, RWKV_DECAY_RANK, RWKV_ICLR_RANK, RWKV_GATE_RANK)), axis=-1)
    w_log = -jax.nn.softplus(-(w0 + jnp.tanh(xw) @ w_decay).astype(jnp.float32)) - 0.5
    decay = jnp.exp(-jnp.exp(w_log))
    a = jax.nn.sigmoid(a0 + xa @ w_iclr)
    g = jax.nn.sigmoid(xg) @ w_gate
    heads = lambda t: t.reshape(B, S, H, N)
    kk = heads(k * k_k).astype(jnp.float32)
    kk = kk / jnp.maximum(jnp.linalg.norm(kk, axis=-1, keepdims=True), 1e-12)
    k = k * (1 + (a - 1) * k_a)
    r_h, k_h, v_h, a_h, w_h = heads(r), heads(k), heads(v), heads(a), heads(decay)
    seq_major = lambda t: jnp.moveaxis(t.astype(jnp.float32), 1, 0)

    def step(state, inp):
        r_t, w_t, k_t, v_t, kk_t, a_t = inp
        sa = jnp.einsum('bhij,bhj->bhi', state, kk_t)
        state = (state * w_t[:, :, None, :]
                 - sa[..., None] * (kk_t * a_t)[:, :, None, :]
                 + v_t[..., None] * k_t[:, :, None, :])
        return state, jnp.einsum('bhij,bhj->bhi', state, r_t)

    s0 = jnp.zeros((B, H, N, N), jnp.float32)
    _, y = lax.scan(step, s0, (seq_major(r_h), seq_major(w_h), seq_major(k_h),
                               seq_major(v_h), seq_major(kk), seq_major(a_h)))
    y = jnp.moveaxis(y, 0, 1)
    mean = jnp.mean(y, axis=-1, keepdims=True)
    var = jnp.mean(jnp.square(y - mean), axis=-1, keepdims=True)
    yn = ((y - mean) * lax.rsqrt(var + RWKV_GN_EPS)).reshape(B, S, RWKV_W).astype(z.dtype) * ln_w + ln_b
    bonus = jnp.sum(r_h * k_h * r_k.reshape(H, N), axis=-1, keepdims=True) * v_h
    return (yn + bonus.reshape(B, S, RWKV_W)) * g


def _mixer_block(h, cos, sin, w_in, mla_q_norm, mla_w_uq, mla_kv_norm, mla_w_ukv,
                 gla_w_alpha, gla_b_alpha, gla_norm,
                 rwkv_mu, rwkv_w0, rwkv_w_decay, rwkv_a0, rwkv_w_iclr, rwkv_w_gate,
                 rwkv_k_k, rwkv_k_a, rwkv_r_k, rwkv_ln_w, rwkv_ln_b, w_branch, w_out):
    B, S, _ = h.shape
    (c_q, c_kv, k_rope, gla_q, gla_k, gla_v, gla_g, gla_a, rwkv_z, gate_logits) = jnp.split(
        h @ w_in, _offsets(IN_SPLITS), axis=-1)
    o_a = _mla_branch(c_q, c_kv, k_rope, mla_q_norm, mla_w_uq, mla_kv_norm, mla_w_ukv, cos, sin)
    o_b = _gla_branch(gla_q, gla_k, gla_v, gla_g, gla_a, gla_w_alpha, gla_b_alpha, gla_norm)
    o_c = _rwkv_branch(rwkv_z, rwkv_mu, rwkv_w0, rwkv_w_decay, rwkv_a0, rwkv_w_iclr, rwkv_w_gate,
                       rwkv_k_k, rwkv_k_a, rwkv_r_k, rwkv_ln_w, rwkv_ln_b)
    y_a = o_a @ w_branch[:MLA_W]
    y_b = o_b @ w_branch[MLA_W:MLA_W + GLA_W]
    y_c = o_c @ w_branch[MLA_W + GLA_W:]
    gates = jax.nn.sigmoid(gate_logits).reshape(B, S, N_BRANCH, D_MODEL)
    merged = gates[:, :, 0] * y_a + gates[:, :, 1] * y_b + gates[:, :, 2] * y_c
    return merged @ w_out


def _mem_attn(h, mem_n, wq, wkv, wo):
    B, S, _ = h.shape
    q = (h @ wq).reshape(B, S, MEM_HEADS, MEM_HD)
    kv = (mem_n @ wkv).reshape(B, mem_n.shape[1], 2, MEM_HEADS, MEM_HD)
    k, v = kv[:, :, 0], kv[:, :, 1]
    s = jnp.einsum('bqhd,bkhd->bhqk', q, k).astype(jnp.float32) * MEM_HD ** -0.5
    p = jax.nn.softmax(s, axis=-1).astype(v.dtype)
    return jnp.einsum('bhqk,bkhd->bqhd', p, v).reshape(B, S, D_MODEL) @ wo


def setup_inputs(seed: int = 0) -> dict:
    key = jax.random.key(seed)
    ks = iter(jax.random.split(key, 48))
    f32 = jnp.float32
    L = DEPTH

    def nrm(shape, fan_in, s=1.0):
        return jax.random.normal(next(ks), shape, f32) * (s * fan_in ** -0.5)

    def gain(shape):
        return 1.0 + 0.05 * jax.random.normal(next(ks), shape, f32)

    def small(shape, s):
        return s * jax.random.normal(next(ks), shape, f32)

    def unif(shape, lo, hi):
        return jax.random.uniform(next(ks), shape, f32, lo, hi)

    x = jax.random.normal(next(ks), (BATCH, SEQ, D_MODEL), f32)
    mem = jax.random.normal(next(ks), (BATCH, N_MEM, D_MODEL), f32)
    start = jax.random.randint(next(ks), (BATCH, 1), 0, 4096, dtype=jnp.int32)
    positions = start + jnp.arange(SEQ, dtype=jnp.int32)[None, :]
    return {
        "x": x,
        "mem": mem,
        "positions": positions,
        "norm_g": gain((L, 8, D_MODEL)),
        "w_ffn_in": nrm((L, 2, D_MODEL, 2 * D_FF), D_MODEL),
        "w_ffn_out": nrm((L, 2, D_FF, D_MODEL), D_FF),
        "w_in": nrm((L, D_MODEL, D_IN), D_MODEL),
        "mla_q_norm": gain((L, MLA_Q_RANK)),
        "mla_w_uq": nrm((L, MLA_Q_RANK, MLA_HEADS * (MLA_NOPE + MLA_ROPE)), MLA_Q_RANK),
        "mla_kv_norm": gain((L, MLA_KV_RANK)),
        "mla_w_ukv": nrm((L, MLA_KV_RANK, MLA_HEADS * (MLA_NOPE + MLA_V)), MLA_KV_RANK),
        "gla_w_alpha": nrm((L, GLA_GATE_RANK, GLA_HEADS * GLA_DK), GLA_GATE_RANK),
        "gla_b_alpha": unif((L, GLA_HEADS * GLA_DK), -1.0, 4.0),
        "gla_norm": gain((L, GLA_DV)),
        "rwkv_mu": unif((L, RWKV_COLS), 0.0, 1.0),
        "rwkv_w0": unif((L, RWKV_W), -4.0, 0.0),
        "rwkv_w_decay": nrm((L, RWKV_DECAY_RANK, RWKV_W), RWKV_DECAY_RANK, 0.5),
        "rwkv_a0": small((L, RWKV_W), 0.1),
        "rwkv_w_iclr": nrm((L, RWKV_ICLR_RANK, RWKV_W), RWKV_ICLR_RANK, 0.5),
        "rwkv_w_gate": nrm((L, RWKV_GATE_RANK, RWKV_W), RWKV_GATE_RANK),
        "rwkv_k_k": 0.85 + small((L, RWKV_W), 0.1),
        "rwkv_k_a": gain((L, RWKV_W)),
        "rwkv_r_k": small((L, RWKV_W), 0.1),
        "rwkv_ln_w": gain((L, RWKV_W)),
        "rwkv_ln_b": small((L, RWKV_W), 0.02),
        "w_branch": nrm((L, BRANCH_W, D_MODEL), RWKV_W),
        "w_out": nrm((L, D_MODEL, D_MODEL), D_MODEL),
        "mem_norm": gain((L, D_MODEL)),
        "mem_wq": nrm((L, D_MODEL, D_MODEL), D_MODEL),
        "mem_wkv": nrm((L, D_MODEL, 2 * D_MODEL), D_MODEL),
        "mem_wo": nrm((L, D_MODEL, D_MODEL), D_MODEL),
    }


def reference(x, mem, positions, norm_g, w_ffn_in, w_ffn_out, w_in,
              mla_q_norm, mla_w_uq, mla_kv_norm, mla_w_ukv,
              gla_w_alpha, gla_b_alpha, gla_norm,
              rwkv_mu, rwkv_w0, rwkv_w_decay, rwkv_a0, rwkv_w_iclr, rwkv_w_gate,
              rwkv_k_k, rwkv_k_a, rwkv_r_k, rwkv_ln_w, rwkv_ln_b,
              w_branch, w_out, mem_norm, mem_wq, mem_wkv, mem_wo):
    inv_freq = ROPE_THETA ** (-jnp.arange(0, MLA_ROPE, 2, dtype=jnp.float32) / MLA_ROPE)
    ang = positions.astype(jnp.float32)[..., None] * inv_freq
    cos, sin = jnp.cos(ang), jnp.sin(ang)
    for l in range(DEPTH):
        ng = norm_g[l]
        x = x + 0.5 * _rmsnorm(_swiglu(_rmsnorm(x, ng[0]), w_ffn_in[l, 0], w_ffn_out[l, 0]), ng[1])
        y = _mixer_block(_rmsnorm(x, ng[2]), cos, sin, w_in[l],
                         mla_q_norm[l], mla_w_uq[l], mla_kv_norm[l], mla_w_ukv[l],
                         gla_w_alpha[l], gla_b_alpha[l], gla_norm[l],
                         rwkv_mu[l], rwkv_w0[l], rwkv_w_decay[l], rwkv_a0[l], rwkv_w_iclr[l],
                         rwkv_w_gate[l], rwkv_k_k[l], rwkv_k_a[l], rwkv_r_k[l],
                         rwkv_ln_w[l], rwkv_ln_b[l], w_branch[l], w_out[l])
        x = x + _rmsnorm(y, ng[3])
        y = _mem_attn(_rmsnorm(x, ng[4]), _rmsnorm(mem, mem_norm[l]), mem_wq[l], mem_wkv[l], mem_wo[l])
        x = x + _rmsnorm(y, ng[5])
        x = x + 0.5 * _rmsnorm(_swiglu(_rmsnorm(x, ng[6]), w_ffn_in[l, 1], w_ffn_out[l, 1]), ng[7])
    return x
```

```python
import contextlib
import numpy as np
import concourse.bass as bass
import concourse.mybir as mybir
from concourse.bass_utils import run_bass_kernel_spmd

F32 = mybir.dt.float32
BF16 = mybir.dt.bfloat16
I32 = mybir.dt.int32
AF = mybir.ActivationFunctionType
ALU = mybir.AluOpType
AX = mybir.AxisListType

D_MODEL = 1024
BATCH = 2
SEQ = 8192
DEPTH = 4
N_MEM = 256
D_FF = 2816
NORM_EPS = 1e-6
D_IN = 6864
N_CORES = 8

SEM_LIMIT = 30000
N_DMA_SEMS = 6


class Sched:
    def __init__(self, nc, stack):
        self.nc = nc
        self.stack = stack
        self.lists = {k: [] for k in ("pe", "act", "dve", "pool", "sp")}
        self.sem = {}
        self.cnt = {}
        self.waited = {}
        self.lastw = {}
        self.readers = {}
        self.nsem = 0
        for e in ("pe", "act", "dve", "pool"):
            self._new_sem(e)
        self.dma_sems = {}
        self.dma_idx = {}
        for q in ("sp", "act", "pool"):
            self.dma_sems[q] = [self._alloc_sem() for _ in range(N_DMA_SEMS)]
            self.dma_idx[q] = 0
        self.dma_uses = {}
        self.out_tokens = []

    def _alloc_sem(self):
        self.nsem += 1
        return self.stack.enter_context(self.nc.semaphore("s%d" % self.nsem))

    def _new_sem(self, e):
        self.sem[e] = self._alloc_sem()
        self.cnt[e] = 0

    def _wait(self, e, tok):
        sem, val, src = tok
        key = (e, id(sem))
        if self.waited.get(key, 0) >= val:
            return
        self.waited[key] = val
        self.lists[e].append(("wait", sem, val))

    def _deps(self, e, reads, writes):
        toks = []
        for k in reads:
            t = self.lastw.get(k)
            if t is not None:
                toks.append(t)
        for k in writes:
            t = self.lastw.get(k)
            if t is not None:
                toks.append(t)
            toks.extend(self.readers.get(k, ()))
        for t in toks:
            if t[2] == "pe" and e == "pe":
                continue
            self._wait(e, t)

    def _commit(self, tok, reads, writes):
        for k in writes:
            self.lastw[k] = tok
            self.readers[k] = []
        for k in reads:
            self.readers.setdefault(k, []).append(tok)

    def op(self, e, fn, reads=(), writes=()):
        self._deps(e, reads, writes)
        if self.cnt[e] >= SEM_LIMIT:
            self._new_sem(e)
        self.cnt[e] += 1
        tok = (self.sem[e], self.cnt[e], e)
        self.lists[e].append(("op", fn, self.sem[e], 1))
        self._commit(tok, reads, writes)
        return tok

    def dma(self, q, fn, reads=(), writes=(), is_out=False):
        self._deps(q, reads, writes)
        i = self.dma_idx[q]
        self.dma_idx[q] += 1
        sem = self.dma_sems[q][i % N_DMA_SEMS]
        uses = self.dma_uses.get(id(sem), 0)
        if uses > 0:
            self._wait(q, (sem, 16 * uses, "dma"))
        self.dma_uses[id(sem)] = uses + 1
        tok = (sem, 16 * (uses + 1), "dma")
        self.lists[q].append(("op", fn, sem, 16))
        self._commit(tok, reads, writes)
        if is_out:
            self.out_tokens.append((q, tok))
        return tok

    def finish(self):
        for q, tok in self.out_tokens:
            self._wait(q, tok)
        self.out_tokens = []
        self.flush()

    def barrier(self):
        toks = []
        for e in ("pe", "act", "dve", "pool"):
            if self.cnt[e] > 0:
                toks.append((self.sem[e], self.cnt[e], e))
        for q in ("sp", "act", "pool"):
            for sem in self.dma_sems[q]:
                u = self.dma_uses.get(id(sem), 0)
                if u > 0:
                    toks.append((sem, 16 * u, "dma"))
        for e in ("pe", "act", "dve", "pool", "sp"):
            for t in toks:
                if t[2] == e:
                    continue
                self._wait(e, t)

    def flush(self):
        nc = self.nc
        lists = self.lists
        self.lists = {k: [] for k in lists}

        def replay(eng, items):
            for it in items:
                if it[0] == "wait":
                    eng.wait_ge(it[1], it[2])
                else:
                    it[1](eng).then_inc(it[2], it[3])

        with nc.Block() as block:
            @block.tensor
            def _(eng):
                replay(eng, lists["pe"])

            @block.scalar
            def _(eng):
                replay(eng, lists["act"])

            @block.vector
            def _(eng):
                replay(eng, lists["dve"])

            @block.gpsimd
            def _(eng):
                replay(eng, lists["pool"])

            @block.sync
            def _(eng):
                replay(eng, lists["sp"])


class Ctx:
    def __init__(self, nc, stack):
        self.nc = nc
        self.stack = stack
        self.s = Sched(nc, stack)
        self.n = 0

    def sb(self, shape, dt, name=None):
        self.n += 1
        return self.stack.enter_context(self.nc.sbuf_tensor(name or ("t%d" % self.n), list(shape), dt))

    def ps(self, shape, dt, name=None):
        self.n += 1
        return self.stack.enter_context(self.nc.psum_tensor(name or ("p%d" % self.n), list(shape), dt))

    def mm(self, out, lhsT, rhs, start, stop, reads, writes):
        return self.s.op("pe", lambda e: e.matmul(out, lhsT, rhs, start=start, stop=stop), reads, writes)

    def tr(self, out, in_, ident, reads, writes):
        return self.s.op("pe", lambda e: e.transpose(out, in_, ident), reads, writes)

    def act(self, out, in_, func, reads, writes, bias=None, scale=None, accum_out=None, eng="act"):
        kw = {}
        if bias is not None:
            kw["bias"] = bias
        if scale is not None:
            kw["scale"] = scale
        if accum_out is not None:
            kw["accum_out"] = accum_out
        return self.s.op(eng, lambda e: e.activation(out, in_, func, **kw), reads, writes)

    def tt(self, eng, out, in0, in1, op, reads, writes):
        return self.s.op(eng, lambda e: e.tensor_tensor(out, in0, in1, op), reads, writes)

    def ts(self, eng, out, in0, s1, s2, op0, op1, reads, writes):
        if op1 is None:
            return self.s.op(eng, lambda e: e.tensor_single_scalar(out, in0, s1, op0), reads, writes)
        return self.s.op(eng, lambda e: e.tensor_scalar(out, in0, s1, s2, op0, op1), reads, writes)

    def stt(self, eng, out, in0, scalar, in1, op0, op1, reads, writes):
        return self.s.op(eng, lambda e: e.scalar_tensor_tensor(out, in0, scalar, in1, op0, op1), reads, writes)

    def copy(self, eng, out, in_, reads, writes):
        if eng == "act":
            return self.s.op(eng, lambda e: e.copy(out, in_), reads, writes)
        return self.s.op(eng, lambda e: e.tensor_copy(out, in_), reads, writes)

    def memset(self, eng, ap, val, writes):
        return self.s.op(eng, lambda e: e.memset(ap, val), (), writes)

    def dma(self, q, out, in_, reads, writes, is_out=False, slow=False):
        if slow:
            return self.s.dma(q, lambda e: e.dma_start(out=out, in_=in_, allow_slow_non_contiguous=True), reads, writes, is_out=is_out)
        return self.s.dma(q, lambda e: e.dma_start(out=out, in_=in_), reads, writes, is_out=is_out)


class Ring:
    def __init__(self, items):
        self.items = items
        self.i = 0

    def next(self):
        it = self.items[self.i % len(self.items)]
        self.i += 1
        return it


def mk_ring(c, n, shape, dt, prefix, psum=False):
    items = []
    for i in range(n):
        t = c.ps(shape, dt) if psum else c.sb(shape, dt)
        items.append((t, "%s%d" % (prefix, i)))
    return Ring(items)


class Tok:
    def __init__(self, c, TB):
        self.c = c
        self.TB = TB
        self.NT = TB // 128
        self.ident = c.sb([128, 128], BF16)
        self.ones = c.sb([128, 128], BF16)
        self.eps = c.sb([128, 1], F32)
        self.xblk = c.sb([128, self.NT, 1024], F32)
        self.hT = c.sb([128, 8, TB], BF16)
        self.big = c.sb([128, 22, TB], BF16)
        self.wres = c.sb([128, 22, 1024], BF16)
        self.wst = mk_ring(c, 3, [128, 8, 512], BF16, "wst")
        self.gains = mk_ring(c, 2, [128, 1024], F32, "gain")
        self.junk = mk_ring(c, 2, [128, 1024], F32, "junk")
        self.xn = mk_ring(c, 2, [128, 1024], BF16, "xn")
        self.ss = mk_ring(c, 4, [128, 8], F32, "ss")
        self.stg = mk_ring(c, 3, [128, 512], F32, "stg")
        self.pmm = mk_ring(c, 6, [128, 512], F32, "pmm", psum=True)
        self.ptr = mk_ring(c, 1, [128, 8, 128], BF16, "ptr", psum=True)

    def setup(self, ident_d):
        c = self.c
        c.dma("pool", self.ident[:], ident_d, [], ["ident"])
        c.memset("dve", self.ones[:], 1.0, ["ones"])
        c.memset("dve", self.eps[:], NORM_EPS, ["eps"])

    def load_gain(self, row_ap):
        c = self.c
        g, gk = self.gains.next()
        D = row_ap.shape[-1]
        c.dma("sp", g[:, 0:D], row_ap.partition_broadcast(128), [], [gk])
        return g, gk

    def rstd(self, ss, ssk, n_parts, D, coef=1.0):
        c = self.c
        if n_parts > 1:
            c.tt("dve", ss[:, 0:1], ss[:, 0:1], ss[:, 1:2], ALU.add, [ssk], [ssk])
        c.act(ss[:, 4:5], ss[:, 0:1], AF.Sqrt, [ssk, "eps"], [ssk], bias=self.eps[:, 0:1], scale=1.0 / D)
        c.s.op("dve", lambda e: e.reciprocal(ss[:, 6:7], ss[:, 4:5]), [ssk], [ssk])
        if coef != 1.0:
            c.ts("dve", ss[:, 6:7], ss[:, 6:7], coef, None, ALU.mult, None, [ssk], [ssk])

    def norm_to_T(self, src, srck, D, gain, gk, dstT, dstk, col0):
        c = self.c
        ss, ssk = self.ss.next()
        junk, jk = self.junk.next()
        xn, xnk = self.xn.next()
        c.memset("pool", ss[:], 0.0, [ssk])
        c.act(junk[:, 0:D], src, AF.Square, [srck, ssk], [jk, ssk], accum_out=ss[:, 0:1])
        self.rstd(ss, ssk, 1, D)
        c.stt("dve", xn[:, 0:D], src, ss[:, 6:7], gain[:, 0:D], ALU.mult, ALU.mult, [srck, ssk, gk], [xnk])
        nk = D // 128
        pt, ptk = self.ptr.next()
        for k in range(nk):
            c.tr(pt[:, k, :], xn[:, k * 128:(k + 1) * 128], self.ident[:], [xnk, "ident"], [ptk])
        c.copy("act", dstT[:, 0:nk, col0:col0 + 128], pt[:, 0:nk, :], [ptk], [dstk])

    def postnorm_res(self, halves, gain, gk, coef, x_ap, xk):
        c = self.c
        ss, ssk = self.ss.next()
        junk, jk = self.junk.next()
        c.memset("pool", ss[:], 0.0, [ssk])
        for i, (p, pk) in enumerate(halves):
            c.act(junk[:, i * 512:(i + 1) * 512], p, AF.Square, [pk, ssk], [jk, ssk], accum_out=ss[:, i:i + 1])
        self.rstd(ss, ssk, 2, 1024, coef)
        for i, (p, pk) in enumerate(halves):
            sl = slice(i * 512, (i + 1) * 512)
            c.stt("dve", junk[:, sl], p, ss[:, 6:7], gain[:, sl], ALU.mult, ALU.mult, [pk, ssk, gk, jk], [jk])
        c.tt("pool", x_ap, x_ap, junk[:], ALU.add, [xk, jk], [xk])

    def load_w(self, w_dram, k0, nk, c0, ncols):
        c = self.c
        w, wk = self.wst.next()
        src = w_dram[k0 * 128:(k0 + nk) * 128, c0:c0 + ncols].rearrange("(k p) n -> p k n", p=128)
        c.dma("pool", w[:, 0:nk, 0:ncols], src, [], [wk])
        return w, wk

    def lin_fm(self, w_dram, k0, nk, c0, ncols, inT, inTk, evac, start=True, stop=True, acc=None):
        c = self.c
        for cb in range(0, ncols, 512):
            nb = min(512, ncols - cb)
            w, wk = self.load_w(w_dram, k0, nk, c0 + cb, nb)
            for j0 in range(0, nb, 128):
                rows = min(128, nb - j0)
                for tg in range(self.TB // 512):
                    p, pk = self.pmm.next()
                    for k in range(nk):
                        c.mm(p[0:rows, :], w[:, k, j0:j0 + rows], inT[:, k, tg * 512:(tg + 1) * 512],
                             k == 0, k == nk - 1, [wk, inTk], [pk])
                    evac((cb + j0) // 128, tg, rows, p, pk)

    def load_wres(self, w_dram, nk, ncols=1024):
        c = self.c
        step = 4
        for k in range(0, nk, step):
            kk = min(step, nk - k)
            src = w_dram[k * 128:(k + kk) * 128, 0:ncols].rearrange("(k p) n -> p k n", p=128)
            c.dma("pool", self.wres[:, k:k + kk, 0:ncols], src, [], ["wres"])

    def lin_tm_res(self, inT, inTk, nk, tile, gain, gk, coef):
        c = self.c
        halves = []
        for h in range(2):
            p, pk = self.pmm.next()
            for k in range(nk):
                c.mm(p[:], inT[:, k, tile * 128:(tile + 1) * 128], self.wres[:, k, h * 512:(h + 1) * 512],
                     k == 0, k == nk - 1, [inTk, "wres"], [pk])
            halves.append((p[:], pk))
        self.postnorm_res(halves, gain, gk, coef, self.xblk[:, tile, :], "x%d" % tile)

    def load_x(self, x_dram, t0):
        c = self.c
        for t in range(self.NT):
            c.dma("sp", self.xblk[:, t, :], x_dram[t0 + t * 128:t0 + (t + 1) * 128, :], [], ["x%d" % t])

    def store_x(self, x_dram, t0):
        c = self.c
        for t in range(self.NT):
            c.dma("sp", x_dram[t0 + t * 128:t0 + (t + 1) * 128, :], self.xblk[:, t, :], ["x%d" % t], [], is_out=True)

    def ffn(self, w_in_d, w_out_d, g_pre_row, g_post_row):
        c = self.c
        g, gk = self.load_gain(g_pre_row)
        for t in range(self.NT):
            self.norm_to_T(self.xblk[:, t, :], "x%d" % t, 1024, g, gk, self.hT, "hT", t * 128)
        self.load_wres(w_out_d, 22)
        sil = mk_ring_cached(self, "sil", 3, [128, 512], F32)
        for cb in range(0, D_FF, 256):
            wg, wgk = self.load_w(w_in_d, 0, 8, cb, 256)
            wu, wuk = self.load_w(w_in_d, 0, 8, D_FF + cb, 256)
            for j in range(2):
                ch = (cb + j * 128) // 128
                for tg in range(self.TB // 512):
                    pg, pgk = self.pmm.next()
                    pu, puk = self.pmm.next()
                    tsl = slice(tg * 512, (tg + 1) * 512)
                    for k in range(8):
                        c.mm(pg[:], wg[:, k, j * 128:(j + 1) * 128], self.hT[:, k, tsl], k == 0, k == 7, [wgk, "hT"], [pgk])
                    for k in range(8):
                        c.mm(pu[:], wu[:, k, j * 128:(j + 1) * 128], self.hT[:, k, tsl], k == 0, k == 7, [wuk, "hT"], [puk])
                    s, sk = sil.next()
                    c.act(s[:], pg[:], AF.Silu, [pgk], [sk])
                    c.tt("dve", self.big[:, ch, tsl], pu[:], s[:], ALU.mult, [puk, sk], ["big", "big2"])
        g2, g2k = self.load_gain(g_post_row)
        for t in range(self.NT):
            self.lin_tm_res(self.big, "big", 22, t, g2, g2k, 0.5)


def mk_ring_cached(obj, name, n, shape, dt, psum=False):
    cache = obj.__dict__.setdefault("_rings", {})
    if name not in cache:
        cache[name] = mk_ring(obj.c, n, shape, dt, name, psum)
    return cache[name]


NPROJ = 3792


def tok_proj(self, w_in_d, g_row, projT_d, gatesT_d, t0):
    c = self.c
    g, gk = self.load_gain(g_row)
    for t in range(self.NT):
        self.norm_to_T(self.xblk[:, t, :], "x%d" % t, 1024, g, gk, self.hT, "hT", t * 128)

    def evac_plain(base):
        def f(j, tg, rows, p, pk):
            s, sk = self.stg.next()
            c.copy("dve", s[0:rows, :], p[0:rows, :], [pk], [sk])
            r0 = base + j * 128
            c.dma("sp", projT_d[r0:r0 + rows, t0 + tg * 512:t0 + (tg + 1) * 512], s[0:rows, :], [sk], [], is_out=True)
        return f

    def evac_gate(j, tg, rows, p, pk):
        s, sk = self.stg.next()
        c.act(s[0:rows, :], p[0:rows, :], AF.Sigmoid, [pk], [sk])
        r0 = j * 128
        c.dma("sp", gatesT_d[r0:r0 + rows, t0 + tg * 512:t0 + (tg + 1) * 512], s[0:rows, :], [sk], [], is_out=True)

    self.lin_fm(w_in_d, 0, 8, 0, 3584, self.hT, "hT", evac_plain(0))
    self.lin_fm(w_in_d, 0, 8, 3584, NPROJ - 3584, self.hT, "hT", evac_plain(3584))
    self.lin_fm(w_in_d, 0, 8, NPROJ, 3072, self.hT, "hT", evac_gate)


def tok_merge(self, og_d, gatesT_d, w_branch_d, w_out_d, g_row, t0):
    c = self.c
    TB = self.TB
    oT = self.big
    for i in range(3):
        for j in range(4):
            c.dma("pool", oT[:, i * 4 + j, :], og_d[j, i * 128:(i + 1) * 128, t0:t0 + TB], [], ["big", "big2"])
    gt = mk_ring_cached(self, "gt", 4, [128, 512], F32)
    acc = mk_ring_cached(self, "acc", 3, [128, 512], F32)
    for cb in range(2):
        ws = [self.load_w(w_branch_d, i * 4, 4, cb * 512, 512) for i in range(3)]
        for fl in range(4):
            fc = cb * 4 + fl
            for tg in range(TB // 512):
                tsl = slice(tg * 512, (tg + 1) * 512)
                terms = []
                for i in range(3):
                    w, wk = ws[i]
                    p, pk = self.pmm.next()
                    for k in range(4):
                        c.mm(p[:], w[:, k, fl * 128:(fl + 1) * 128], oT[:, i * 4 + k, tsl], k == 0, k == 3, [wk, "big"], [pk])
                    gtile, gtk = gt.next()
                    c.dma("sp", gtile[:], gatesT_d[i * 1024 + fc * 128:i * 1024 + (fc + 1) * 128, t0 + tg * 512:t0 + (tg + 1) * 512], [], [gtk])
                    a, ak = acc.next()
                    c.tt("dve", a[:], p[:], gtile[:], ALU.mult, [pk, gtk], [ak])
                    terms.append((a, ak))
                (a0, k0), (a1, k1), (a2, k2) = terms
                c.tt("pool", a0[:], a0[:], a1[:], ALU.add, [k0, k1], [k0])
                c.tt("pool", self.hT[:, fc, tsl], a0[:], a2[:], ALU.add, [k0, k2], ["hT"])
    self.load_wres(w_out_d, 8)
    g, gk = self.load_gain(g_row)
    for t in range(self.NT):
        self.lin_tm_res(self.hT, "hT", 8, t, g, gk, 1.0)


def tok_mem_setup(self, mem_d, mem_norm_row, wkv_d):
    c = self.c
    self.memx = c.sb([128, 2, 1024], F32)
    self.memT = c.sb([128, 8, 256], BF16)
    self.kT = c.sb([128, 8, 256], BF16)
    self.vM = c.sb([128, 2, 1024], BF16)
    g, gk = self.load_gain(mem_norm_row)
    for t in range(2):
        c.dma("sp", self.memx[:, t, :], mem_d[t * 128:(t + 1) * 128, :], [], ["memx%d" % t])
        self.norm_to_T(self.memx[:, t, :], "memx%d" % t, 1024, g, gk, self.memT, "memT", t * 128)
    for cb in range(2):
        w, wk = self.load_w(wkv_d, 0, 8, cb * 512, 512)
        for j in range(4):
            p, pk = self.pmm.next()
            for k in range(8):
                c.mm(p[:, 0:256], w[:, k, j * 128:(j + 1) * 128], self.memT[:, k, :], k == 0, k == 7, [wk, "memT"], [pk])
            c.act(self.kT[:, cb * 4 + j, :], p[:, 0:256], AF.Copy, [pk], ["kT"], scale=1.0 / 16.0)
    for hf in range(2):
        w, wk = self.load_w(wkv_d, 0, 8, 1024 + hf * 512, 512)
        for mt in range(2):
            p, pk = self.pmm.next()
            for k in range(8):
                c.mm(p[:], self.memT[:, k, mt * 128:(mt + 1) * 128], w[:, k, :], k == 0, k == 7, [wk, "memT"], [pk])
            c.copy("dve", self.vM[:, mt, hf * 512:(hf + 1) * 512], p[:], [pk], ["vM"])


def tok_mem_attn(self, wq_d, wo_d, g_pre_row, g_post_row):
    c = self.c
    TB = self.TB
    g, gk = self.load_gain(g_pre_row)
    for t in range(self.NT):
        self.norm_to_T(self.xblk[:, t, :], "x%d" % t, 1024, g, gk, self.hT, "hT", t * 128)

    def evac_q(j, tg, rows, p, pk):
        c.copy("act", self.big[:, j, tg * 512:(tg + 1) * 512], p[:], [pk], ["big"])

    self.lin_fm(wq_d, 0, 8, 0, 1024, self.hT, "hT", evac_q)
    pT = mk_ring_cached(self, "pT", 4, [128, 512], BF16)
    rden = mk_ring_cached(self, "rden", 2, [128, 512], F32)
    for h in range(4):
        for tg in range(TB // 512):
            tsl = slice(tg * 512, (tg + 1) * 512)
            pts = []
            for mt in range(2):
                p, pk = self.pmm.next()
                for ch in range(2):
                    c.mm(p[:], self.kT[:, 2 * h + ch, mt * 128:(mt + 1) * 128], self.big[:, 2 * h + ch, tsl],
                         ch == 0, ch == 1, ["kT", "big"], [pk])
                e, ek = pT.next()
                c.act(e[:], p[:], AF.Exp, [pk], [ek])
                pts.append((e, ek))
            p, pk = self.pmm.next()
            for mt in range(2):
                c.mm(p[:], self.ones[:], pts[mt][0][:], mt == 0, mt == 1, ["ones", pts[mt][1]], [pk])
            rd, rdk = rden.next()
            c.s.op("dve", lambda e, rd=rd, p=p: e.reciprocal(rd[:], p[:]), [pk], [rdk])
            for dch in range(2):
                p, pk = self.pmm.next()
                for mt in range(2):
                    c.mm(p[:], self.vM[:, mt, h * 256 + dch * 128:h * 256 + (dch + 1) * 128], pts[mt][0][:],
                         mt == 0, mt == 1, ["vM", pts[mt][1]], [pk])
                c.tt("dve", self.big[:, 8 + 2 * h + dch, tsl], p[:], rd[:], ALU.mult, [pk, rdk], ["big2"])
    self.load_wres(wo_d, 8)
    g2, g2k = self.load_gain(g_post_row)
    for t in range(self.NT):
        c = self.c
        halves = []
        for hh in range(2):
            p, pk = self.pmm.next()
            for k in range(8):
                c.mm(p[:], self.big[:, 8 + k, t * 128:(t + 1) * 128], self.wres[:, k, hh * 512:(hh + 1) * 512],
                     k == 0, k == 7, ["big2", "wres"], [pk])
            halves.append((p[:], pk))
        self.postnorm_res(halves, g2, g2k, 1.0, self.xblk[:, t, :], "x%d" % t)


Tok.proj = tok_proj
Tok.merge = tok_merge
Tok.mem_setup = tok_mem_setup
Tok.mem_attn = tok_mem_attn


def build_tok(T, TB, do_c, do_a):
    nc = bass.Bass("TRN2", target_bir_lowering=False)

    def din(name, shape):
        return nc.dram_tensor(name, list(shape), F32, kind="ExternalInput").ap()

    def dout(name, shape):
        return nc.dram_tensor(name, list(shape), F32, kind="ExternalOutput").ap()

    x_in = din("x_in", [T, 1024])
    idn = din("idn", [128, 128])
    x_out = dout("x_out", [T, 1024])
    if do_c:
        og = din("og", [4, 384, T])
        gates_in = din("gates_in", [3072, T])
        ng_c = din("ng_c", [8, 1024])
        w_branch = din("w_branch", [1536, 1024])
        w_out = din("w_out", [1024, 1024])
        mem = din("mem", [256, 1024])
        mem_norm = din("mem_norm", [1, 1024])
        wq = din("mem_wq", [1024, 1024])
        wkv = din("mem_wkv", [1024, 2048])
        wo = din("mem_wo", [1024, 1024])
        f2_in = din("f2_in", [1024, 2 * D_FF])
        f2_out = din("f2_out", [D_FF, 1024])
    if do_a:
        ng_a = din("ng_a", [8, 1024])
        f1_in = din("f1_in", [1024, 2 * D_FF])
        f1_out = din("f1_out", [D_FF, 1024])
        w_in = din("w_in", [1024, D_IN])
        projT = dout("projT", [NPROJ, T])
        gatesT = dout("gatesT", [3072, T])
    with contextlib.ExitStack() as st:
        c = Ctx(nc, st)
        tk = Tok(c, TB)
        tk.setup(idn)
        if do_c:
            tk.mem_setup(mem, mem_norm, wkv)
        for t0 in range(0, T, TB):
            tk.load_x(x_in, t0)
            if do_c:
                tk.merge(og, gates_in, w_branch, w_out, ng_c[3:4, :], t0)
                tk.mem_attn(wq, wo, ng_c[4:5, :], ng_c[5:6, :])
                tk.ffn(f2_in, f2_out, ng_c[6:7, :], ng_c[7:8, :])
            if do_a:
                tk.ffn(f1_in, f1_out, ng_a[0:1, :], ng_a[1:2, :])
                tk.proj(w_in, ng_a[2:3, :], projT, gatesT, t0)
            tk.store_x(x_out, t0)
        c.s.finish()
    return nc


R_CQ, R_CKV, R_KR = 0, 256, 384
R_GQ, R_GK, R_GV, R_GG, R_GA = 416, 480, 544, 672, 800
R_RR, R_RK, R_RV, R_XW, R_XA, R_XG = 816, 944, 1072, 1200, 1264, 1328
R_TOT = 1488
MLA_SCALE = 96 ** -0.5
TWO_PI = 6.283185307179586
CW1 = 6.28125
CW2 = TWO_PI - 6.28125


class Phase:
    def __init__(self, c):
        self.c = c

    def __enter__(self):
        self.st = contextlib.ExitStack()
        self.st.__enter__()
        self.save = self.c.stack
        self.c.stack = self.st
        self.c.s.barrier()
        return self

    def __exit__(self, *a):
        self.c.s.barrier()
        self.c.s.flush()
        self.c.stack = self.save
        return self.st.__exit__(*a)


def pg_rows(pg, r0, n, t0, nt):
    BW = pg.shape[2]
    rk = t0 // BW
    o = t0 % BW
    assert o + nt <= BW
    return pg[rk, r0:r0 + n, o:o + nt]


def seq_mla(c, S, pg, wd, oT_d):
    BK = min(1024, S)
    with Phase(c):
        pmm = mk_ring(c, 4, [128, 512], F32, "qmm", psum=True)
        QT = c.sb([96, 2, S], BF16)
        KT = c.sb([96, 2, S], BF16)
        VA = c.sb([128, S // 128, 2, 65], BF16)
        ident32 = c.sb([128, 128], F32)
        ones = c.sb([128, 128], BF16)
        wuq = c.sb([128, 2, 2, 96], BF16)
        wuqs = c.sb([128, 2, 2, 96], BF16)
        wk0 = c.sb([128, 2, 96], BF16)
        wk1 = c.sb([32, 2, 96], BF16)
        wv = c.sb([128, 128], BF16)
        qn = c.sb([128, 2], F32)
        kvn = c.sb([128, 1], F32)
        invf = c.sb([16, 1], F32)
        cst = c.sb([128, 4], F32)
        tri = c.sb([128, 128], BF16)
        c.dma("pool", wuq[:], wd["wuq"].rearrange("(k p) h n -> p k h n", p=128), [], ["mw"])
        c.dma("pool", wuqs[:], wd["wuqs"].rearrange("(k p) h n -> p k h n", p=128), [], ["mw"])
        c.dma("pool", wk0[:], wd["wk0"], [], ["mw"])
        c.dma("pool", wk1[:], wd["wk1"], [], ["mw"])
        c.dma("pool", wv[:], wd["wv"], [], ["mw"])
        c.dma("pool", tri[:], wd["tri"], [], ["mw"])
        c.dma("sp", ident32[:], wd["ident"], [], ["ident32"])
        c.dma("sp", qn[:], wd["qn"].rearrange("(k p) -> p k", p=128), [], ["mw"], slow=True)
        c.dma("sp", kvn[:], wd["kvn"].rearrange("(k p) -> p k", p=128), [], ["mw"], slow=True)
        c.dma("sp", invf[:], wd["invf"].rearrange("(p o) -> p o", o=1), [], ["mw"], slow=True)
        c.memset("dve", ones[:], 1.0, ["ones"])
        c.memset("dve", cst[:, 0:1], NORM_EPS, ["mw"])
        c.memset("dve", VA[:, :, :, 64:65], 1.0, ["VA"])
        with Phase(c):
            cq32 = c.sb([128, 2, BK], F32)
            sq = c.sb([128, 2, BK], BF16)
            cqn = c.sb([128, 2, BK], BF16)
            ckv32 = c.sb([128, BK], F32)
            ckvn = c.sb([128, BK], BF16)
            kr = c.sb([32, BK], BF16)
            rs = mk_ring(c, 2, [128, 512], F32, "rs")
            posi = c.sb([16, BK], I32)
            ang = c.sb([16, BK], F32)
            kf = c.sb([16, BK], F32)
            ki = c.sb([16, BK], I32)
            tmp = c.sb([16, BK], F32)
            cs = c.sb([16, 3, BK], F32)
            CF = c.sb([96, BK], F32)
            SF = c.sb([96, BK], F32)
            t1 = mk_ring(c, 2, [96, 512], F32, "t1")
            t2 = mk_ring(c, 2, [96, 512], F32, "t2")
            c.memset("dve", CF[0:64, :], 1.0, ["CF"])
            c.memset("dve", SF[0:64, :], 0.0, ["SF"])
            for b0 in range(0, S, BK):
                c.dma("sp", posi[:], wd["pos"][0:1, b0:b0 + BK].partition_broadcast(16), [], ["posi"])
                c.copy("dve", ang[:], posi[:], ["posi"], ["ang"])
                c.ts("dve", ang[:], ang[:], invf[:, 0:1], None, ALU.mult, None, ["ang", "mw"], ["ang"])
                c.ts("dve", kf[:], ang[:], 1.0 / TWO_PI, None, ALU.mult, None, ["ang"], ["kf"])
                c.copy("dve", ki[:], kf[:], ["kf"], ["ki"])
                c.copy("dve", kf[:], ki[:], ["ki"], ["kf"])
                c.stt("dve", tmp[:], kf[:], -CW1, ang[:], ALU.mult, ALU.add, ["kf", "ang"], ["tmp"])
                c.stt("dve", tmp[:], kf[:], -CW2, tmp[:], ALU.mult, ALU.add, ["kf", "tmp"], ["tmp"])
                c.ts("dve", kf[:], tmp[:], float(np.pi), None, ALU.is_gt, None, ["tmp"], ["kf"])
                c.stt("dve", tmp[:], kf[:], -TWO_PI, tmp[:], ALU.mult, ALU.add, ["kf", "tmp"], ["tmp"])
                c.act(cs[:, 1, :], tmp[:], AF.Sin, ["tmp"], ["cs"])
                c.ts("dve", cs[:, 2, :], cs[:, 1, :], -1.0, None, ALU.mult, None, ["cs"], ["cs"])
                c.ts("dve", tmp[:], tmp[:], float(np.pi / 2), None, ALU.add, None, ["tmp"], ["tmp"])
                c.ts("dve", kf[:], tmp[:], float(np.pi), None, ALU.is_gt, None, ["tmp"], ["kf"])
                c.stt("dve", tmp[:], kf[:], -TWO_PI, tmp[:], ALU.mult, ALU.add, ["kf", "tmp"], ["tmp"])
                c.act(cs[:, 0, :], tmp[:], AF.Sin, ["tmp"], ["cs"])
                c.dma("sp", CF[64:80, :], cs[:, 0, :], ["cs"], ["CF"])
                c.dma("sp", CF[80:96, :], cs[:, 0, :], ["cs"], ["CF"])
                c.dma("sp", SF[64:80, :], cs[:, 2, :], ["cs"], ["SF"])
                c.dma("sp", SF[80:96, :], cs[:, 1, :], ["cs"], ["SF"])
                for k in range(2):
                    c.dma("sp", cq32[:, k, :], pg_rows(pg, R_CQ + k * 128, 128, b0, BK), [], ["cq32"])
                c.dma("sp", ckv32[:], pg_rows(pg, R_CKV, 128, b0, BK), [], ["ckv32"])
                c.dma("pool", kr[:], pg_rows(pg, R_KR, 32, b0, BK), [], ["kr"])
                c.tt("pool", sq[:], cq32[:], cq32[:], ALU.mult, ["cq32"], ["sq"])
                for tg in range(BK // 512):
                    tsl = slice(tg * 512, (tg + 1) * 512)
                    p, pk = pmm.next()
                    for k in range(2):
                        c.mm(p[:], ones[:], sq[:, k, tsl], k == 0, k == 1, ["ones", "sq"], [pk])
                    r, rk = rs.next()
                    c.act(r[:], p[:], AF.Sqrt, [pk, "mw"], [rk], bias=cst[:, 0:1], scale=1.0 / 256)
                    c.s.op("dve", lambda e, r=r: e.reciprocal(r[:], r[:]), [rk], [rk])
                    for k in range(2):
                        c.stt("dve", cqn[:, k, tsl], cq32[:, k, tsl], qn[:, k:k + 1], r[:], ALU.mult, ALU.mult,
                              ["cq32", "mw", rk], ["cqn"])
                c.tt("pool", sq[:, 0, :], ckv32[:], ckv32[:], ALU.mult, ["ckv32"], ["sq"])
                for tg in range(BK // 512):
                    tsl = slice(tg * 512, (tg + 1) * 512)
                    p, pk = pmm.next()
                    c.mm(p[:], ones[:], sq[:, 0, tsl], True, True, ["ones", "sq"], [pk])
                    r, rk = rs.next()
                    c.act(r[:], p[:], AF.Sqrt, [pk, "mw"], [rk], bias=cst[:, 0:1], scale=1.0 / 128)
                    c.s.op("dve", lambda e, r=r: e.reciprocal(r[:], r[:]), [rk], [rk])
                    c.stt("dve", ckvn[:, tsl], ckv32[:, tsl], kvn[:, 0:1], r[:], ALU.mult, ALU.mult,
                          ["ckv32", "mw", rk], ["ckvn"])
                for h in range(2):
                    for tg in range(BK // 512):
                        tsl = slice(tg * 512, (tg + 1) * 512)
                        gsl = slice(b0 + tg * 512, b0 + (tg + 1) * 512)
                        for which in range(2):
                            pa, pak = pmm.next()
                            pb, pbk = pmm.next()
                            if which == 0:
                                for k in range(2):
                                    c.mm(pa[0:96, :], wuq[:, k, h, :], cqn[:, k, tsl], k == 0, k == 1, ["mw", "cqn"], [pak])
                                for k in range(2):
                                    c.mm(pb[0:96, :], wuqs[:, k, h, :], cqn[:, k, tsl], k == 0, k == 1, ["mw", "cqn"], [pbk])
                            else:
                                c.mm(pa[0:96, :], wk0[:, h, :], ckvn[:, tsl], True, False, ["mw", "ckvn"], [pak])
                                c.mm(pa[0:96, :], wk1[:, 0, :], kr[:, tsl], False, True, ["mw", "kr"], [pak])
                                c.mm(pb[0:96, :], wk1[:, 1, :], kr[:, tsl], True, True, ["mw", "kr"], [pbk])
                            a, ak = t1.next()
                            b, bk = t2.next()
                            sc = MLA_SCALE if which == 0 else 1.0
                            c.stt("dve", a[:], pa[0:96, :], sc, CF[:, tsl], ALU.mult, ALU.mult, [pak, "CF"], [ak])
                            c.stt("dve", b[:], pb[0:96, :], sc, SF[:, tsl], ALU.mult, ALU.mult, [pbk, "SF"], [bk])
                            dst, dk = (QT, "QT") if which == 0 else (KT, "KT")
                            c.tt("pool", dst[:, h, gsl], a[:], b[:], ALU.add, [ak, bk], [dk])
                for t in range(BK // 128):
                    p, pk = pmm.next()
                    c.mm(p[:, 0:128], ckvn[:, t * 128:(t + 1) * 128], wv[:], True, True, ["ckvn", "mw"], [pk])
                    gt = (b0 // 128) + t
                    c.copy("act", VA[:, gt, :, 0:64], p[:, 0:128].rearrange("p (h d) -> p h d", h=2), [pk], ["VA"])
        with Phase(c):
            pT = mk_ring(c, 3, [128, 512], BF16, "mpT")
            acc = mk_ring(c, 4, [128, 512], F32, "macc", psum=True)
            osb = mk_ring(c, 8, [128, 128], F32, "mosb")
            rc = mk_ring(c, 4, [128, 8], F32, "mrc")
            otr = mk_ring(c, 2, [128, 128], F32, "motr")
            for qb in range(S // 512):
                otiles = [osb.next() for _ in range(4)]
                for h in range(2):
                    accs = [acc.next() for _ in range(4)]
                    nkt = 4 * qb + 4
                    for kt in range(nkt):
                        d = kt - 4 * qb
                        cs0 = max(0, d) * 128
                        N = 512 - cs0
                        p, pk = pmm.next()
                        c.mm(p[:, 0:N], KT[:, h, kt * 128:(kt + 1) * 128], QT[:, h, qb * 512 + cs0:(qb + 1) * 512],
                             True, True, ["KT", "QT"], [pk])
                        e, ek = pT.next()
                        c.act(e[:, 0:N], p[:, 0:N], AF.Exp, [pk], [ek])
                        if d >= 0:
                            c.tt("pool", e[:, 0:128], e[:, 0:128], tri[:], ALU.mult, [ek, "mw"], [ek])
                        for qs in range(max(0, d), 4):
                            a, ak = accs[qs]
                            c.mm(a[:, 0:65], e[:, qs * 128 - cs0:(qs + 1) * 128 - cs0], VA[:, kt, h, :],
                                 kt == 0, kt == 4 * qb + qs, [ek, "VA"], [ak])
                    for qs in range(4):
                        a, ak = accs[qs]
                        o, ok = otiles[qs]
                        r, rk = rc.next()
                        c.s.op("dve", lambda e_, r=r, a=a: e_.reciprocal(r[:, 0:1], a[:, 64:65]), [ak], [rk])
                        c.ts("dve", o[:, h * 64:(h + 1) * 64], a[:, 0:64], r[:, 0:1], None, ALU.mult, None, [ak, rk], [ok])
                for qs in range(4):
                    o, ok = otiles[qs]
                    p, pk = pmm.next()
                    c.tr(p[:, 0:128], o[:], ident32[:], [ok, "ident32"], [pk])
                    t, tk = otr.next()
                    c.copy("act", t[:], p[:, 0:128], [pk], [tk])
                    q0 = qb * 512 + qs * 128
                    c.dma("sp", oT_d[0:128, q0:q0 + 128], t[:], [tk], [], is_out=True)


def mla_host_weights(j, w_uq, w_ukv, q_norm, kv_norm, positions_b):
    f = np.float32
    wuq = np.zeros((256, 2, 96), f)
    wuqs = np.zeros((256, 2, 96), f)
    wk0 = np.zeros((128, 2, 96), f)
    wk1 = np.zeros((32, 2, 96), f)
    wv = np.zeros((128, 128), f)
    for h in range(2):
        hh = 2 * j + h
        wq = w_uq[:, hh * 96:(hh + 1) * 96]
        wuq[:, h, :] = wq
        wuqs[:, h, 64:80] = wq[:, 80:96]
        wuqs[:, h, 80:96] = wq[:, 64:80]
        wk0[:, h, 0:64] = w_ukv[:, hh * 128:hh * 128 + 64]
        wv[:, h * 64:(h + 1) * 64] = w_ukv[:, hh * 128 + 64:hh * 128 + 128]
    for i in range(32):
        wk1[i, 0, 64 + i] = 1.0
    for i in range(16):
        wk1[16 + i, 1, 64 + i] = 1.0
        wk1[i, 1, 80 + i] = 1.0
    invf = (np.float32(10000.0) ** (-np.arange(0, 32, 2, dtype=np.float32) / np.float32(32))).astype(f)
    return dict(wuq=wuq, wuqs=wuqs, wk0=wk0, wk1=wk1, wv=wv, tri=np.triu(np.ones((128, 128), f)),
                qn=np.ascontiguousarray(q_norm, f), kvn=np.ascontiguousarray(kv_norm, f), invf=invf,
                pos=np.ascontiguousarray(positions_b.reshape(1, -1), np.int32), ident=np.eye(128, dtype=f))


def seq_gla(c, S, pg, wd, oT_d):
    BK = min(1024, S)
    with Phase(c):
        pmm = mk_ring(c, 4, [128, 512], F32, "gmm", psum=True)
        ptb = mk_ring(c, 2, [128, 1024], BF16, "gtb", psum=True)
        ident = c.sb([128, 128], BF16)
        ident32 = c.sb([128, 128], F32)
        tri = c.sb([128, 128], BF16)
        walpha = c.sb([16, 64], BF16)
        nb = c.sb([64, 2], F32)
        gng = c.sb([128, 128], F32)
        cst = c.sb([128, 4], F32)
        rm = c.sb([64, BK], F32)
        qT = c.sb([64, BK], F32)
        kT = c.sb([64, BK], F32)
        aT = c.sb([16, BK], BF16)
        vT = c.sb([128, BK], BF16)
        gT = c.sb([128, BK], F32)
        l1 = c.sb([64, BK], F32)
        cs = c.sb([64, BK], F32)
        eb = c.sb([64, BK], F32)
        enb = c.sb([64, BK], F32)
        qt = c.sb([64, BK], BF16)
        kt = c.sb([64, BK], BF16)
        st32 = c.sb([64, 128], F32)
        stb = mk_ring(c, 2, [64, 128], BF16, "gstb")
        vtm = mk_ring(c, 2, [128, 128], BF16, "gvtm")
        sg = mk_ring(c, 2, [128, 128], F32, "gsg")
        att = mk_ring(c, 2, [128, 128], BF16, "gatt")
        ktm = mk_ring(c, 2, [128, 64], BF16, "gktm")
        ss = mk_ring(c, 3, [128, 8], F32, "gss")
        junk = mk_ring(c, 2, [128, 128], F32, "gjunk")
        on = mk_ring(c, 2, [128, 128], F32, "gon")
        otr = mk_ring(c, 2, [128, 128], F32, "gotr")
        c.dma("pool", ident[:], wd["ident"], [], ["ident"])
        c.dma("sp", ident32[:], wd["ident"], [], ["ident32"])
        c.dma("pool", tri[:], wd["tri"], [], ["gw"])
        c.dma("pool", walpha[:], wd["walpha"], [], ["gw"])
        c.dma("sp", nb[:, 0:1], wd["balpha"].rearrange("(p o) -> p o", o=1), [], ["gw"], slow=True)
        c.ts("dve", nb[:, 1:2], nb[:, 0:1], -1.0, None, ALU.mult, None, ["gw"], ["gw"])
        c.dma("sp", gng[:], wd["gnorm"][0:1, :].partition_broadcast(128), [], ["gw"])
        c.memset("dve", cst[:, 0:1], NORM_EPS, ["gw"])
        c.memset("dve", cst[:, 1:2], 1.0, ["gw"])
        c.memset("dve", rm[:], 1.0, ["rm"])
        c.memset("dve", rm[:].rearrange("p (c t) -> p c t", t=128)[:, :, 0:1], 0.0, ["rm"])
        c.memset("dve", st32[:], 0.0, ["st32"])
        sb0, sbk0 = stb.next()
        c.memset("dve", sb0[:], 0.0, [sbk0])
        cur_stb = (sb0, sbk0)
        for b0 in range(0, S, BK):
            c.dma("sp", qT[:], pg_rows(pg, R_GQ, 64, b0, BK), [], ["gq"])
            c.dma("sp", kT[:], pg_rows(pg, R_GK, 64, b0, BK), [], ["gk"])
            c.dma("pool", aT[:], pg_rows(pg, R_GA, 16, b0, BK), [], ["ga"])
            c.dma("pool", vT[:], pg_rows(pg, R_GV, 128, b0, BK), [], ["gv"])
            c.dma("sp", gT[:], pg_rows(pg, R_GG, 128, b0, BK), [], ["gg"])
            for tg in range(BK // 512):
                tsl = slice(tg * 512, (tg + 1) * 512)
                p, pk = pmm.next()
                c.mm(p[0:64, :], walpha[:], aT[:, tsl], True, True, ["gw", "ga"], [pk])
                c.act(l1[:, tsl], p[0:64, :], AF.Exp, [pk, "gw"], ["l1"], bias=nb[:, 1:2], scale=-1.0)
            c.act(l1[:], l1[:], AF.Ln, ["l1", "gw"], ["l1"], bias=cst[0:64, 1:2])
            c.s.op("dve", lambda e: e.tensor_tensor_scan(cs[:], rm[:], l1[:], 0.0, ALU.mult, ALU.add), ["rm", "l1"], ["cs"])
            c.act(eb[:], cs[:], AF.Exp, ["cs"], ["eb"], scale=-1.0 / 16)
            c.act(enb[:], cs[:], AF.Exp, ["cs"], ["enb"], scale=1.0 / 16)
            c.stt("dve", qt[:], qT[:], 0.125, eb[:], ALU.mult, ALU.mult, ["gq", "eb"], ["qt"])
            c.tt("pool", kt[:], kT[:], enb[:], ALU.mult, ["gk", "enb"], ["kt"])
            for t in range(BK // 128):
                tl = slice(t * 128, (t + 1) * 128)
                pb, pbk = ptb.next()
                c.tr(pb[:, 0:128], vT[:, tl], ident[:], ["gv", "ident"], [pbk])
                v, vk = vtm.next()
                c.copy("act", v[:], pb[:, 0:128], [pbk], [vk])
                p, pk = pmm.next()
                c.tr(p[:, 0:128], gT[:, tl], ident32[:], ["gg", "ident32"], [pk])
                s, sk = sg.next()
                c.act(s[:], p[:, 0:128], AF.Silu, [pk], [sk])
                p, pk = pmm.next()
                c.mm(p[:, 0:128], kt[:, tl], qt[:, tl], True, True, ["kt", "qt"], [pk])
                a, ak = att.next()
                c.tt("dve", a[:], p[:, 0:128], tri[:], ALU.mult, [pk, "gw"], [ak])
                pb, pbk = ptb.next()
                c.tr(pb[:, 0:64], kt[:, tl], ident[0:64, 0:64], ["kt", "ident"], [pbk])
                km, kmk = ktm.next()
                c.copy("act", km[:], pb[:, 0:64], [pbk], [kmk])
                sbf, sbfk = cur_stb
                po, pok = pmm.next()
                c.mm(po[:, 0:128], a[:], v[:], True, False, [ak, vk], [pok])
                c.mm(po[:, 0:128], qt[:, tl], sbf[:], False, True, ["qt", sbfk], [pok])
                p2, p2k = pmm.next()
                c.mm(p2[0:64, 0:128], km[:], v[:], True, True, [kmk, vk], [p2k])
                el = eb[:, t * 128 + 127:t * 128 + 128]
                c.ts("dve", st32[:], st32[:], el, None, ALU.mult, None, ["st32", "eb"], ["st32"])
                c.stt("dve", st32[:], p2[0:64, 0:128], el, st32[:], ALU.mult, ALU.add, [p2k, "eb", "st32"], ["st32"])
                nsb, nsbk = stb.next()
                c.copy("pool", nsb[:], st32[:], ["st32"], [nsbk])
                cur_stb = (nsb, nsbk)
                s_, ssk = ss.next()
                j_, jk = junk.next()
                c.memset("pool", s_[:], 0.0, [ssk])
                c.act(j_[:], po[:, 0:128], AF.Square, [pok, ssk], [jk, ssk], accum_out=s_[:, 0:1])
                c.act(s_[:, 4:5], s_[:, 0:1], AF.Sqrt, [ssk, "gw"], [ssk], bias=cst[:, 0:1], scale=1.0 / 128)
                c.s.op("dve", lambda e, s_=s_: e.reciprocal(s_[:, 6:7], s_[:, 4:5]), [ssk], [ssk])
                o_, ok = on.next()
                c.stt("dve", o_[:], po[:, 0:128], s_[:, 6:7], gng[:], ALU.mult, ALU.mult, [pok, ssk, "gw"], [ok])
                c.tt("pool", o_[:], o_[:], s[:], ALU.mult, [ok, sk], [ok])
                p, pk = pmm.next()
                c.tr(p[:, 0:128], o_[:], ident32[:], [ok, "ident32"], [pk])
                tt_, tk = otr.next()
                c.copy("act", tt_[:], p[:, 0:128], [pk], [tk])
                c.dma("sp", oT_d[128:256, b0 + t * 128:b0 + (t + 1) * 128], tt_[:], [tk], [], is_out=True)


def gla_host_weights(j, w_alpha, b_alpha, gnorm):
    f = np.float32
    return dict(walpha=np.ascontiguousarray(w_alpha[:, j * 64:(j + 1) * 64], f),
                balpha=np.ascontiguousarray(b_alpha[j * 64:(j + 1) * 64], f),
                gnorm=np.ascontiguousarray(gnorm.reshape(1, 128), f),
                tri=np.triu(np.ones((128, 128), f)), ident=np.eye(128, dtype=f))


RW_DEC = 0.6065306597126334


def seq_rwkv(c, S, pg, wds, oT_d):
    BK = min(512, S)
    NCH = BK // 64
    NG = NCH // 4
    with Phase(c):
        pm = mk_ring(c, 6, [128, 512], F32, "rmm", psum=True)
        pY, pYk = mk_ring(c, 1, [128, 512], F32, "rpy", psum=True).next()
        w0 = wds[0]
        ident32 = c.sb([128, 128], F32)
        ones = c.sb([64, 64], F32)
        msk = c.sb([64, 5, 4, 64], F32)
        rm = c.sb([64, BK], F32)
        cst = c.sb([128, 4], F32)
        c.dma("sp", ident32[:], w0["ident"], [], ["ident32"])
        c.dma("sp", msk[:], w0["masks"], [], ["msk"])
        c.memset("dve", ones[:], 1.0, ["ones"])
        c.memset("dve", rm[:], 1.0, ["rm"])
        c.memset("dve", rm[:].rearrange("p (c t) -> p c t", t=64)[:, :, 0:1], 0.0, ["rm"])
        c.memset("dve", cst[:, 0:1], 1e-24, ["cst"])
        c.memset("dve", cst[:, 1:2], 64e-5, ["cst"])
        mu_s = c.sb([128, 4], F32)
        for i, (nm, n) in enumerate((("mu_w", 64), ("mu_a", 64), ("mu_g0", 128), ("mu_g1", 32))):
            c.dma("sp", mu_s[0:n, i:i + 1], w0[nm].rearrange("(p o) -> p o", o=1), [], ["mus"], slow=True)
        hx = [c.sb([n, BK + 1], F32) for n in (64, 64, 128, 32)]
        dx = c.sb([128, BK], F32)
        zx = c.sb([128, BK], F32)
        txw = c.sb([64, BK], BF16)
        xab = c.sb([64, BK], BF16)
        sxg0 = c.sb([128, BK], BF16)
        sxg1 = c.sb([32, BK], BF16)
        for t in hx:
            c.memset("dve", t[:, 0:1], 0.0, ["hx"])
        heads = []
        for hl in range(2):
            wd = wds[hl]
            H = {}
            H["cols"] = c.sb([64, 16], F32)
            for i, nm in enumerate(("mu_r", "mu_k", "mu_v", "w0", "a0", "k_k", "k_a", "r_k", "ln_w", "ln_b")):
                c.dma("sp", H["cols"][:, i:i + 1], wd[nm].rearrange("(p o) -> p o", o=1), [], ["hw%d" % hl], slow=True)
            c.ts("dve", H["cols"][:, 10:11], H["cols"][:, 6:7], -1.0, 1.0, ALU.mult, ALU.add, ["hw%d" % hl], ["hw%d" % hl])
            H["wdec"] = c.sb([64, 64], BF16)
            H["wicl"] = c.sb([64, 64], BF16)
            H["wg0"] = c.sb([128, 64], BF16)
            H["wg1"] = c.sb([32, 64], BF16)
            c.dma("pool", H["wdec"][:], wd["w_decay"], [], ["hw%d" % hl])
            c.dma("pool", H["wicl"][:], wd["w_iclr"], [], ["hw%d" % hl])
            c.dma("pool", H["wg0"][:], wd["w_gate"][0:128, :], [], ["hw%d" % hl])
            c.dma("pool", H["wg1"][:], wd["w_gate"][128:160, :], [], ["hw%d" % hl])
            H["hz"] = [c.sb([64, BK + 1], F32) for _ in range(3)]
            for t in H["hz"]:
                c.memset("dve", t[:, 0:1], 0.0, ["hz%d" % hl])
            H["T"] = c.sb([64, 64], F32)
            c.memset("dve", H["T"][:], 0.0, ["T%d" % hl])
            heads.append(H)
        F = lambda: c.sb([64, BK], F32)
        rp, kp, vp = F(), F(), F()
        dz = F()
        sgu, cs_, csx, p_, pinv, pex, pend = F(), F(), F(), F(), F(), F(), F()
        a_, g_, kk, kk2, kap, tt_, k2, b_, rk, bon = F(), F(), F(), F(), F(), F(), F(), F(), F(), F()
        KRt = c.sb([64, NCH, 2, 64], F32)
        BKt = c.sb([64, NCH, 2, 64], F32)
        bch = F()
        kch = F()
        Xr = mk_ring(c, 2, [64, 4, 128], F32, "rX")
        ZZr = mk_ring(c, 2, [64, 4, 128], F32, "rZZ")
        BBr = mk_ring(c, 2, [64, 4, 128], F32, "rBB")
        KVr = mk_ring(c, 2, [64, 4, 128], F32, "rKV")
        BkTr = mk_ring(c, 2, [64, 4, 64], F32, "rBkT")
        OPSr = mk_ring(c, 2, [64, 4, 4, 64], F32, "rOPS")
        ysb = c.sb([64, 512], F32)
        yc = c.sb([64, 512], F32)
        y2 = c.sb([64, 512], F32)
        rsd = c.sb([64, 512], F32)

        def v3(t):
            return t[:].rearrange("p (c t) -> p c t", t=64)

        def bank3(p):
            return p[0:64, :].rearrange("p (c n) -> p c n", n=128)

        for b0 in range(0, S, BK):
            srcs = ((R_XW, 64), (R_XA, 64), (R_XG, 128), (R_XG + 128, 32))
            for i, (r0, n) in enumerate(srcs):
                h = hx[i]
                if b0 > 0:
                    c.copy("pool", h[:, 0:1], h[:, BK:BK + 1], ["hx"], ["hx"])
                c.dma("sp", h[:, 1:BK + 1], pg_rows(pg, r0, n, b0, BK), [], ["hx"])
                c.tt("dve", dx[0:n, :], h[:, 0:BK], h[:, 1:BK + 1], ALU.subtract, ["hx"], ["dx"])
                c.stt("dve", zx[0:n, :], dx[0:n, :], mu_s[0:n, i:i + 1], h[:, 1:BK + 1], ALU.mult, ALU.add,
                      ["dx", "hx", "mus"], ["zx"])
                if i == 0:
                    c.act(txw[:], zx[0:64, :], AF.Tanh, ["zx"], ["txw"])
                elif i == 1:
                    c.copy("act", xab[:], zx[0:64, :], ["zx"], ["xab"])
                elif i == 2:
                    c.act(sxg0[:], zx[0:128, :], AF.Sigmoid, ["zx"], ["sxg0"])
                else:
                    c.act(sxg1[:], zx[0:32, :], AF.Sigmoid, ["zx"], ["sxg1"])
            for hl in range(2):
                H = heads[hl]
                hw = "hw%d" % hl
                cols = H["cols"]
                Tk = "T%d" % hl
                for i, (r0, dst) in enumerate(((R_RR, rp), (R_RK, kp), (R_RV, vp))):
                    h = H["hz"][i]
                    if b0 > 0:
                        c.copy("pool", h[:, 0:1], h[:, BK:BK + 1], ["hz%d" % hl], ["hz%d" % hl])
                    c.dma("sp", h[:, 1:BK + 1], pg_rows(pg, r0 + hl * 64, 64, b0, BK), [], ["hz%d" % hl])
                    c.tt("dve", dz[:], h[:, 0:BK], h[:, 1:BK + 1], ALU.subtract, ["hz%d" % hl], ["dz"])
                    c.stt("dve", dst[:], dz[:], cols[:, i:i + 1], h[:, 1:BK + 1], ALU.mult, ALU.add,
                          ["dz", "hz%d" % hl, hw], ["rkv%d" % i])
                for tg in range(BK // 512):
                    tsl = slice(tg * 512, (tg + 1) * 512)
                    p, pk = pm.next()
                    c.mm(p[0:64, :], H["wdec"][:], txw[:, tsl], True, True, [hw, "txw"], [pk])
                    c.act(sgu[:, tsl], p[0:64, :], AF.Sigmoid, [pk, hw], ["sgu"], bias=cols[:, 3:4])
                    p, pk = pm.next()
                    c.mm(p[0:64, :], H["wicl"][:], xab[:, tsl], True, True, [hw, "xab"], [pk])
                    c.act(a_[:, tsl], p[0:64, :], AF.Sigmoid, [pk, hw], ["a"], bias=cols[:, 4:5])
                    p, pk = pm.next()
                    c.mm(p[0:64, :], H["wg0"][:], sxg0[:, tsl], True, False, [hw, "sxg0"], [pk])
                    c.mm(p[0:64, :], H["wg1"][:], sxg1[:, tsl], False, True, [hw, "sxg1"], [pk])
                    c.copy("dve", g_[:, tsl], p[0:64, :], [pk], ["g"])
                c.s.op("dve", lambda e: e.tensor_tensor_scan(cs_[:], rm[:], sgu[:], 0.0, ALU.mult, ALU.add), ["rm", "sgu"], ["cs"])
                c.tt("pool", csx[:], cs_[:], sgu[:], ALU.subtract, ["cs", "sgu"], ["csx"])
                c.act(p_[:], cs_[:], AF.Exp, ["cs"], ["p"], scale=-RW_DEC)
                c.act(pinv[:], cs_[:], AF.Exp, ["cs"], ["pinv"], scale=RW_DEC)
                c.act(pex[:], csx[:], AF.Exp, ["csx"], ["pex"], scale=-RW_DEC)
                c.tt("dve", v3(csx), v3(cs_)[:, :, 63:64].to_broadcast([64, NCH, 64]), v3(cs_), ALU.subtract, ["cs", "pex"], ["csx"])
                c.act(pend[:], csx[:], AF.Exp, ["csx"], ["pend"], scale=-RW_DEC)
                c.ts("dve", kk[:], kp[:], cols[:, 5:6], None, ALU.mult, None, ["rkv1", hw], ["kk"])
                c.tt("pool", kk2[:], kk[:], kk[:], ALU.mult, ["kk"], ["kk2"])
                for tg in range(BK // 512):
                    tsl = slice(tg * 512, (tg + 1) * 512)
                    p, pk = pm.next()
                    c.mm(p[0:64, :], ones[:], kk2[:, tsl], True, True, ["ones", "kk2"], [pk])
                    c.act(kk2[:, tsl], p[0:64, :], AF.Sqrt, [pk, "cst", "kk2"], ["kk2"], bias=cst[0:64, 0:1])
                c.s.op("dve", lambda e: e.reciprocal(kk2[:], kk2[:]), ["kk2"], ["kk2"])
                c.tt("dve", kap[:], kk[:], kk2[:], ALU.mult, ["kk", "kk2"], ["kap"])
                c.ts("dve", tt_[:], a_[:], cols[:, 6:7], cols[:, 10:11], ALU.mult, ALU.add, ["a", hw], ["tt"])
                c.tt("pool", k2[:], kp[:], tt_[:], ALU.mult, ["rkv1", "tt"], ["k2"])
                c.tt("pool", b_[:], kap[:], a_[:], ALU.mult, ["kap", "a"], ["b"])
                c.stt("dve", rk[:], rp[:], cols[:, 7:8], k2[:], ALU.mult, ALU.mult, ["rkv0", hw, "k2"], ["rk"])
                for tg in range(BK // 512):
                    tsl = slice(tg * 512, (tg + 1) * 512)
                    p, pk = pm.next()
                    c.mm(p[0:64, :], ones[:], rk[:, tsl], True, True, ["ones", "rk"], [pk])
                    c.copy("act", bon[:, tsl], p[0:64, :], [pk], ["bon"])
                c.tt("dve", KRt[:, :, 0, :], v3(kap), v3(pex), ALU.mult, ["kap", "pex"], ["KRt"])
                c.tt("pool", KRt[:, :, 1, :], v3(rp), v3(p_), ALU.mult, ["rkv0", "p"], ["KRt"])
                c.tt("dve", BKt[:, :, 0, :], v3(b_), v3(pinv), ALU.mult, ["b", "pinv"], ["BKt"])
                c.tt("pool", BKt[:, :, 1, :], v3(k2), v3(pinv), ALU.mult, ["k2", "pinv"], ["BKt"])
                c.tt("dve", bch[:], b_[:], pend[:], ALU.mult, ["b", "pend"], ["bch"])
                c.tt("pool", kch[:], k2[:], pend[:], ALU.mult, ["k2", "pend"], ["kch"])
                for g in range(NG):
                    X, Xk = Xr.next()
                    ZZ, ZZk = ZZr.next()
                    BB, BBk = BBr.next()
                    KV, KVk = KVr.next()
                    BkT, BkTk = BkTr.next()
                    OPS, OPSk = OPSr.next()
                    ch0 = g * 4
                    p1, p1k = pm.next()
                    p2, p2k = pm.next()
                    for cc in range(4):
                        ch = ch0 + cc
                        cl = slice(ch * 64, (ch + 1) * 64)
                        c.tr(bank3(p1)[:, cc, 0:64], KRt[:, ch, 0, :], ident32[0:64, 0:64], ["KRt", "ident32"], [p1k])
                        c.tr(bank3(p1)[:, cc, 64:128], bch[:, cl], ident32[0:64, 0:64], ["bch", "ident32"], [p1k])
                        c.tr(bank3(p2)[:, cc, 0:64], kch[:, cl], ident32[0:64, 0:64], ["kch", "ident32"], [p2k])
                        c.tr(bank3(p2)[:, cc, 64:128], vp[:, cl], ident32[0:64, 0:64], ["rkv2", "ident32"], [p2k])
                    c.copy("act", X[:, :, 0:64], bank3(p1)[:, :, 0:64], [p1k], [Xk])
                    c.copy("act", BB[:, :, 64:128], bank3(p1)[:, :, 64:128], [p1k], [BBk])
                    c.copy("dve", KV[:], bank3(p2), [p2k], [KVk])
                    pa, pak = pm.next()
                    pb, pbk = pm.next()
                    pc, pck = pm.next()
                    for cc in range(4):
                        ch = ch0 + cc
                        c.mm(bank3(pa)[:, cc, :], KRt[:, ch, 0, :], BKt[:, ch, :, :].rearrange("p a t -> p (a t)"),
                             True, True, ["KRt", "BKt"], [pak])
                        c.mm(bank3(pb)[:, cc, :], BKt[:, ch, 0, :], KRt[:, ch, :, :].rearrange("p a t -> p (a t)"),
                             True, True, ["KRt", "BKt"], [pbk])
                        c.mm(bank3(pc)[:, cc, 0:64], BKt[:, ch, 1, :], KRt[:, ch, 1, :], True, True, ["KRt", "BKt"], [pck])
                    c.tt("dve", ZZ[:, :, 0:64], bank3(pa)[:, :, 0:64], msk[:, 0, :, :], ALU.mult, [pak, "msk"], [ZZk])
                    c.tt("dve", X[:, :, 64:128], bank3(pa)[:, :, 64:128], msk[:, 1, :, :], ALU.mult, [pak, "msk"], [Xk])
                    c.tt("dve", ZZ[:, :, 64:128], bank3(pb)[:, :, 0:64], msk[:, 2, :, :], ALU.mult, [pbk, "msk"], [ZZk])
                    c.tt("dve", BB[:, :, 0:64], bank3(pb)[:, :, 64:128], msk[:, 3, :, :], ALU.mult, [pbk, "msk"], [BBk])
                    c.tt("dve", BkT[:], bank3(pc)[:, :, 0:64], msk[:, 4, :, :], ALU.mult, [pck, "msk"], [BkTk])
                    for lvl in range(6):
                        px, pxk = pm.next()
                        for cc in range(4):
                            c.mm(bank3(px)[:, cc, :], ZZ[:, cc, 64:128], X[:, cc, :], True, True, [ZZk, Xk], [pxk])
                        if lvl < 5:
                            pz, pzk = pm.next()
                            for cc in range(4):
                                c.mm(bank3(pz)[:, cc, 0:64], ZZ[:, cc, 64:128], ZZ[:, cc, 0:64], True, True, [ZZk], [pzk])
                                c.mm(bank3(pz)[:, cc, 64:128], ZZ[:, cc, 0:64], ZZ[:, cc, 64:128], True, True, [ZZk], [pzk])
                        X2, X2k = Xr.next()
                        c.tt("dve", X2[:], X[:], bank3(px), ALU.add, [Xk, pxk], [X2k])
                        X, Xk = X2, X2k
                        if lvl < 5:
                            ZZ2, ZZ2k = ZZr.next()
                            c.copy("act", ZZ2[:], bank3(pz), [pzk], [ZZ2k])
                            ZZ, ZZk = ZZ2, ZZ2k
                    pR, pRk = pm.next()
                    pG, pGk = pm.next()
                    for cc in range(4):
                        c.mm(bank3(pR)[:, cc, :], X[:, cc, 0:64], BB[:, cc, :], True, True, [Xk, BBk], [pRk])
                        c.mm(bank3(pG)[:, cc, :], X[:, cc, 64:128], BB[:, cc, :], True, True, [Xk, BBk], [pGk])
                    c.tt("dve", OPS[:, :, 0, :], KRt[:, ch0:ch0 + 4, 1, :], bank3(pR)[:, :, 0:64], ALU.subtract, ["KRt", pRk], [OPSk])
                    c.tt("dve", OPS[:, :, 1, :], BkT[:], bank3(pG)[:, :, 0:64], ALU.subtract, [BkTk, pGk], [OPSk])
                    for cc in range(4):
                        ch = ch0 + cc
                        c.stt("dve", OPS[:, cc, 2, :], ident32[0:64, 0:64], p_[:, ch * 64 + 63:ch * 64 + 64],
                              bank3(pR)[:, cc, 64:128], ALU.mult, ALU.subtract, ["ident32", "p", pRk], [OPSk])
                    c.tt("dve", OPS[:, :, 3, :], KV[:, :, 0:64], bank3(pG)[:, :, 64:128], ALU.subtract, [KVk, pGk], [OPSk])
                    T = H["T"]
                    for cc in range(4):
                        ch = ch0 + cc
                        yo = (ch % 8) * 64
                        c.mm(pY[0:64, yo:yo + 64], T[:], OPS[:, cc, 0, :], True, False, [Tk, OPSk], [pYk])
                        c.mm(pY[0:64, yo:yo + 64], KV[:, cc, 64:128], OPS[:, cc, 1, :], False, True, [KVk, OPSk], [pYk])
                        ps_, psk = pm.next()
                        c.mm(ps_[0:64, 0:64], OPS[:, cc, 2, :], T[:], True, False, [OPSk, Tk], [psk])
                        c.mm(ps_[0:64, 0:64], OPS[:, cc, 3, :], KV[:, cc, 64:128], False, True, [OPSk, KVk], [psk])
                        c.copy("act", T[:], ps_[0:64, 0:64], [psk], [Tk])
                    if (ch0 + 4) % 8 == 0:
                        t0 = (ch0 + 4) * 64 - 512
                        tsl = slice(t0, t0 + 512)
                        c.copy("act", ysb[:], pY[0:64, :], [pYk], ["ysb"])
                        p, pk = pm.next()
                        c.mm(p[0:64, :], ones[:], ysb[:], True, True, ["ones", "ysb"], [pk])
                        c.stt("dve", yc[:], p[0:64, :], -1.0 / 64, ysb[:], ALU.mult, ALU.add, [pk, "ysb"], ["yc"])
                        c.tt("pool", y2[:], yc[:], yc[:], ALU.mult, ["yc"], ["y2"])
                        p, pk = pm.next()
                        c.mm(p[0:64, :], ones[:], y2[:], True, True, ["ones", "y2"], [pk])
                        c.act(rsd[:], p[0:64, :], AF.Sqrt, [pk, "cst"], ["rsd"], bias=cst[0:64, 1:2], scale=1.0 / 64)
                        c.s.op("dve", lambda e: e.reciprocal(rsd[:], rsd[:]), ["rsd"], ["rsd"])
                        c.tt("dve", yc[:], yc[:], rsd[:], ALU.mult, ["yc", "rsd"], ["yc"])
                        c.ts("dve", yc[:], yc[:], cols[:, 8:9], cols[:, 9:10], ALU.mult, ALU.add, ["yc", hw], ["yc"])
                        c.tt("pool", y2[:], bon[:, tsl], vp[:, tsl], ALU.mult, ["bon", "rkv2"], ["y2"])
                        c.tt("pool", yc[:], yc[:], y2[:], ALU.add, ["yc", "y2"], ["yc"])
                        c.tt("pool", yc[:], yc[:], g_[:, tsl], ALU.mult, ["yc", "g"], ["yc"])
                        c.dma("sp", oT_d[256 + hl * 64:256 + (hl + 1) * 64, b0 + t0:b0 + t0 + 512], yc[:], ["yc"], [], is_out=True)


def rwkv_host_weights(j, W):
    f = np.float32
    sl = np.tril(np.ones((64, 64), f), -1)
    su = np.triu(np.ones((64, 64), f), 1)
    iu = np.triu(np.ones((64, 64), f))
    masks = np.stack([np.stack([m] * 4, 0) for m in (-sl, sl, -su, iu, iu)], 0)
    masks = np.ascontiguousarray(masks.transpose(2, 0, 1, 3))
    mu = np.asarray(W["rwkv_mu"], f)
    out = []
    for hl in range(2):
        h = 2 * j + hl
        cs = slice(h * 64, (h + 1) * 64)
        d = dict(mu_r=mu[0:512][cs], mu_k=mu[512:1024][cs], mu_v=mu[1024:1536][cs],
                 w0=W["rwkv_w0"][cs], a0=W["rwkv_a0"][cs], k_k=W["rwkv_k_k"][cs], k_a=W["rwkv_k_a"][cs],
                 r_k=W["rwkv_r_k"][cs], ln_w=W["rwkv_ln_w"][cs], ln_b=W["rwkv_ln_b"][cs],
                 w_decay=W["rwkv_w_decay"][:, cs], w_iclr=W["rwkv_w_iclr"][:, cs], w_gate=W["rwkv_w_gate"][:, cs])
        d = {k: np.ascontiguousarray(v, f) for k, v in d.items()}
        out.append(d)
    out[0].update(mu_w=np.ascontiguousarray(mu[1536:1600]), mu_a=np.ascontiguousarray(mu[1600:1664]),
                  mu_g0=np.ascontiguousarray(mu[1664:1792]), mu_g1=np.ascontiguousarray(mu[1792:1824]),
                  masks=masks, ident=np.eye(128, dtype=f))
    return out


def _decl(nc, prefix, d):
    out = {}
    for k, v in d.items():
        dt = I32 if v.dtype == np.int32 else F32
        out[k] = nc.dram_tensor(prefix + k, list(v.shape), dt, kind="ExternalInput").ap()
    return out


def build_seq(S, BW, ex_m, ex_g, ex_r):
    nc = bass.Bass("TRN2", target_bir_lowering=False)
    pg = nc.dram_tensor("pg", [S // BW, R_TOT, BW], F32, kind="ExternalInput").ap()
    wm = _decl(nc, "m_", ex_m)
    wg = _decl(nc, "g_", ex_g)
    wr = [_decl(nc, "r%d_" % i, ex_r[i]) for i in range(2)]
    oT = nc.dram_tensor("oT", [384, S], F32, kind="ExternalOutput").ap()
    with contextlib.ExitStack() as st:
        c = Ctx(nc, st)
        seq_mla(c, S, pg, wm, oT)
        seq_gla(c, S, pg, wg, oT)
        seq_rwkv(c, S, pg, wr, oT)
        c.s.finish()
    return nc


def seq_inputs(pgh, hm, hg, hr):
    ins = {"pg": pgh}
    ins.update({"m_" + k: v for k, v in hm.items()})
    ins.update({"g_" + k: v for k, v in hg.items()})
    for i in range(2):
        ins.update({"r%d_%s" % (i, k): v for k, v in hr[i].items()})
    return ins


def slice_pg(projTs, j):
    BW = projTs[0].shape[1]
    out = np.empty((len(projTs), R_TOT, BW), np.float32)
    Z = 1968
    for r, P in enumerate(projTs):
        o = out[r]
        o[0:416] = P[0:416]
        o[R_GQ:R_GQ + 64] = P[416 + 64 * j:416 + 64 * (j + 1)]
        o[R_GK:R_GK + 64] = P[672 + 64 * j:672 + 64 * (j + 1)]
        o[R_GV:R_GV + 128] = P[928 + 128 * j:928 + 128 * (j + 1)]
        o[R_GG:R_GG + 128] = P[1440 + 128 * j:1440 + 128 * (j + 1)]
        o[R_GA:R_GA + 16] = P[1952:1968]
        o[R_RR:R_RR + 128] = P[Z + 128 * j:Z + 128 * (j + 1)]
        o[R_RK:R_RK + 128] = P[Z + 512 + 128 * j:Z + 512 + 128 * (j + 1)]
        o[R_RV:R_RV + 128] = P[Z + 1024 + 128 * j:Z + 1024 + 128 * (j + 1)]
        o[R_XW:R_XW + 64] = P[Z + 1536:Z + 1600]
        o[R_XA:R_XA + 64] = P[Z + 1600:Z + 1664]
        o[R_XG:R_XG + 160] = P[Z + 1664:Z + 1824]
    return out


def kernel(**inputs):
    f = np.float32
    I = {k: np.asarray(v) for k, v in inputs.items()}
    B, S, D = I["x"].shape
    T = (B * S) // N_CORES
    GP = N_CORES // B
    xs = [np.ascontiguousarray(I["x"].reshape(B * S, D)[c * T:(c + 1) * T], f) for c in range(N_CORES)]
    idn = np.eye(128, dtype=f)
    cores = list(range(N_CORES))
    A = lambda a: np.ascontiguousarray(a, f)

    def tok_inputs(l_c, l_a, xs_, ogs, gates):
        maps = []
        for c in cores:
            b = c // GP
            m = {"x_in": xs_[c], "idn": idn}
            if l_c is not None:
                l = l_c
                m.update(og=ogs[c], gates_in=gates[c], ng_c=A(I["norm_g"][l]), w_branch=A(I["w_branch"][l]),
                         w_out=A(I["w_out"][l]), mem=A(I["mem"][b]), mem_norm=A(I["mem_norm"][l].reshape(1, -1)),
                         mem_wq=A(I["mem_wq"][l]), mem_wkv=A(I["mem_wkv"][l]), mem_wo=A(I["mem_wo"][l]),
                         f2_in=A(I["w_ffn_in"][l, 1]), f2_out=A(I["w_ffn_out"][l, 1]))
            if l_a is not None:
                l = l_a
                m.update(ng_a=A(I["norm_g"][l]), f1_in=A(I["w_ffn_in"][l, 0]), f1_out=A(I["w_ffn_out"][l, 0]),
                         w_in=A(I["w_in"][l]))
            maps.append(m)
        return maps

    progs = {}

    def tok_prog(do_c, do_a):
        key = (do_c, do_a)
        if key not in progs:
            progs[key] = build_tok(T, 512, do_c, do_a)
        return progs[key]

    res = run_bass_kernel_spmd(tok_prog(False, True), tok_inputs(None, 0, xs, None, None), core_ids=cores)
    xs = [r["x_out"] for r in res.results]
    projTs = [r["projT"] for r in res.results]
    gates = [r["gatesT"] for r in res.results]
    seq_nc = None
    L = I["norm_g"].shape[0]
    for l in range(L):
        maps = []
        for c in cores:
            b, j = c // GP, c % GP
            pgh = slice_pg(projTs[b * GP:(b + 1) * GP], j)
            hm = mla_host_weights(j, I["mla_w_uq"][l], I["mla_w_ukv"][l], I["mla_q_norm"][l], I["mla_kv_norm"][l],
                                  I["positions"][b])
            hg = gla_host_weights(j, I["gla_w_alpha"][l], I["gla_b_alpha"][l], I["gla_norm"][l])
            hr = rwkv_host_weights(j, {k: I[k][l] for k in I if k.startswith("rwkv_")})
            if seq_nc is None:
                seq_nc = build_seq(S, T, hm, hg, hr)
            maps.append(seq_inputs(pgh, hm, hg, hr))
        res = run_bass_kernel_spmd(seq_nc, maps, core_ids=cores)
        oTs = [r["oT"] for r in res.results]
        ogs = []
        for c in cores:
            b, r = c // GP, c % GP
            ogs.append(np.ascontiguousarray(np.stack([oTs[b * GP + jj][:, r * T:(r + 1) * T] for jj in range(GP)], 0), f))
        last = l == L - 1
        res = run_bass_kernel_spmd(tok_prog(True, not last), tok_inputs(l, None if last else l + 1, xs, ogs, gates), core_ids=cores)
        xs = [r["x_out"] for r in res.results]
        if not last:
            projTs = [r["projT"] for r in res.results]
            gates = [r["gatesT"] for r in res.results]
    return np.concatenate(xs, 0).reshape(B, S, D).astype(f)
```
